# Optimizing a Trainium2 kernel written in Bass

```python
import math
import jax, jax.numpy as jnp
from jax import lax
import numpy as np

D_MODEL = 1024
BATCH = 4
SEQ = 8192
DEPTH = 1

D_MIX = D_MODEL
D_LRU = D_MIX // 2
LRU_HEADS = 8
LRU_HEAD_DIM = D_LRU // LRU_HEADS
CONV_WIDTH = 4
LRU_C = 8.0
D_FOURIER = D_MIX - D_LRU
FOURIER_GROUPS = 4
FOURIER_GROUP_DIM = D_FOURIER // FOURIER_GROUPS
D_IN_PROJ = 2 * D_LRU + D_FOURIER
N_EXPERTS = 16
CAPACITY_FACTOR = 2
D_FF_EXPERT = 2 * D_MODEL
EPS = 1e-6

kernel_name = "hybrid_rglru_fourier_ecmoe_encoder"


def rmsnorm(x, g):
    xf = x.astype(jnp.float32)
    y = xf * lax.rsqrt(jnp.mean(xf * xf, axis=-1, keepdims=True) + EPS)
    return (y * g.astype(jnp.float32)).astype(x.dtype)


def centred_depthwise_conv(u, w, b):
    s = u.shape[1]
    left = CONV_WIDTH // 2
    right = CONV_WIDTH - 1 - left
    up = jnp.pad(u, ((0, 0), (left, right), (0, 0)))
    out = b
    for k in range(CONV_WIDTH):
        out = out + w[k] * up[:, k:k + s, :]
    return out


def _linear_recurrence_combine(left, right):
    a1, b1 = left
    a2, b2 = right
    return a1 * a2, a2 * b1 + b2


def rglru(u, w_a, b_a, w_x, b_x, lam, reverse):
    bsz, s, _ = u.shape
    uf = u.astype(jnp.float32)
    uh = uf.reshape(bsz, s, LRU_HEADS, LRU_HEAD_DIM)
    r = jax.nn.sigmoid(jnp.einsum('bshi,hij->bshj', uh, w_a.astype(jnp.float32)).reshape(bsz, s, D_LRU) + b_a)
    i = jax.nn.sigmoid(jnp.einsum('bshi,hij->bshj', uh, w_x.astype(jnp.float32)).reshape(bsz, s, D_LRU) + b_x)
    log_a = LRU_C * r * jax.nn.log_sigmoid(lam.astype(jnp.float32))
    a = jnp.exp(log_a)
    mult = jnp.sqrt(-jnp.expm1(2.0 * log_a))
    bterm = mult * (i * uf)
    _, h = lax.associative_scan(_linear_recurrence_combine, (a, bterm), axis=1, reverse=reverse)
    return h


def fourier_mix(u):
    bsz, s, _ = u.shape
    f = u.astype(jnp.float32).reshape(bsz, s, FOURIER_GROUPS, FOURIER_GROUP_DIM)
    y = jnp.fft.fft2(f, axes=(1, 3), norm='ortho').real
    return y.reshape(bsz, s, D_FOURIER)


def expert_choice_moe(h, w_router, w_gate, w_up, w_down):
    bsz, s, d = h.shape
    cap = CAPACITY_FACTOR * s // N_EXPERTS
    logits = jnp.einsum('bsd,de->bse', h.astype(jnp.float32), w_router.astype(jnp.float32))
    aff = jax.nn.softmax(logits, axis=-1)
    scores = jnp.transpose(aff, (0, 2, 1))
    gate, idx = lax.top_k(scores, cap)
    bidx = jnp.arange(bsz)[:, None, None]
    xin = h[bidx, idx]
    g = jnp.einsum('becd,edf->becf', xin, w_gate)
    u = jnp.einsum('becd,edf->becf', xin, w_up)
    eo = jnp.einsum('becf,efd->becd', jax.nn.silu(g) * u, w_down)
    contrib = gate.astype(eo.dtype)[..., None] * eo
    out = jnp.zeros((bsz, s, d), dtype=eo.dtype).at[bidx, idx].add(contrib)
    return out


def setup_inputs(seed: int = 0) -> dict:
    key = jax.random.key(seed)
    ks = jax.random.split(key, 24)
    f32 = jnp.float32
    nrm = lambda k, shape, scale: jax.random.normal(k, shape, f32) * scale

    def lam_init(k):
        u = jax.random.uniform(k, (D_LRU,), f32, 0.9, 0.999)
        return jnp.log(u) - jnp.log1p(-u)

    return {
        "x": nrm(ks[0], (BATCH, SEQ, D_MODEL), 1.0),
        "norm1_g": 1.0 + nrm(ks[1], (D_MODEL,), 0.02),
        "w_in": nrm(ks[2], (D_MODEL, D_IN_PROJ), D_MODEL ** -0.5),
        "conv_w": nrm(ks[3], (CONV_WIDTH, D_LRU), CONV_WIDTH ** -0.5),
        "conv_b": nrm(ks[4], (D_LRU,), 0.01),
        "lru_wa_f": nrm(ks[5], (LRU_HEADS, LRU_HEAD_DIM, LRU_HEAD_DIM), LRU_HEAD_DIM ** -0.5),
        "lru_ba_f": nrm(ks[6], (D_LRU,), 0.01),
        "lru_wx_f": nrm(ks[7], (LRU_HEADS, LRU_HEAD_DIM, LRU_HEAD_DIM), LRU_HEAD_DIM ** -0.5),
        "lru_bx_f": nrm(ks[8], (D_LRU,), 0.01),
        "lru_lam_f": lam_init(ks[9]),
        "lru_wa_b": nrm(ks[10], (LRU_HEADS, LRU_HEAD_DIM, LRU_HEAD_DIM), LRU_HEAD_DIM ** -0.5),
        "lru_ba_b": nrm(ks[11], (D_LRU,), 0.01),
        "lru_wx_b": nrm(ks[12], (LRU_HEADS, LRU_HEAD_DIM, LRU_HEAD_DIM), LRU_HEAD_DIM ** -0.5),
        "lru_bx_b": nrm(ks[13], (D_LRU,), 0.01),
        "lru_lam_b": lam_init(ks[14]),
        "w_out": nrm(ks[15], (D_MIX, D_MODEL), D_MIX ** -0.5),
        "norm2_g": 1.0 + nrm(ks[16], (D_MODEL,), 0.02),
        "w_router": nrm(ks[17], (D_MODEL, N_EXPERTS), D_MODEL ** -0.5),
        "w_gate": nrm(ks[18], (N_EXPERTS, D_MODEL, D_FF_EXPERT), D_MODEL ** -0.5),
        "w_up": nrm(ks[19], (N_EXPERTS, D_MODEL, D_FF_EXPERT), D_MODEL ** -0.5),
        "w_down": nrm(ks[20], (N_EXPERTS, D_FF_EXPERT, D_MODEL), D_FF_EXPERT ** -0.5),
        "normf_g": 1.0 + nrm(ks[21], (D_MODEL,), 0.02),
    }


def reference(x, norm1_g, w_in, conv_w, conv_b,
              lru_wa_f, lru_ba_f, lru_wx_f, lru_bx_f, lru_lam_f,
              lru_wa_b, lru_ba_b, lru_wx_b, lru_bx_b, lru_lam_b,
              w_out, norm2_g, w_router, w_gate, w_up, w_down, normf_g):
    for _ in range(DEPTH):
        h = rmsnorm(x, norm1_g)
        p = jnp.einsum('bsd,dn->bsn', h, w_in)
        lru_x = p[..., :D_LRU]
        lru_gate = p[..., D_LRU:2 * D_LRU]
        four_in = p[..., 2 * D_LRU:]
        c = centred_depthwise_conv(lru_x, conv_w, conv_b)
        h_f = rglru(c, lru_wa_f, lru_ba_f, lru_wx_f, lru_bx_f, lru_lam_f, reverse=False)
        h_b = rglru(c, lru_wa_b, lru_ba_b, lru_wx_b, lru_bx_b, lru_lam_b, reverse=True)
        y_lru = (jax.nn.gelu(lru_gate.astype(jnp.float32)) * (h_f + h_b)).astype(x.dtype)
        y_four = fourier_mix(four_in).astype(x.dtype)
        mixed = jnp.concatenate([y_lru, y_four], axis=-1)
        x = x + jnp.einsum('bsm,md->bsd', mixed, w_out)
        h2 = rmsnorm(x, norm2_g)
        x = x + expert_choice_moe(h2, w_router, w_gate, w_up, w_down).astype(x.dtype)
    return rmsnorm(x, normf_g)
```

```python
import math
from contextlib import ExitStack

import numpy as np
import concourse.bass as bass
import concourse.mybir as mybir
from concourse.bass_utils import run_bass_kernel_spmd

F32 = mybir.dt.float32
BF16 = mybir.dt.bfloat16
I32 = mybir.dt.int32
U32 = mybir.dt.uint32
AF = mybir.ActivationFunctionType
ALU = mybir.AluOpType
AX = mybir.AxisListType

S = 8192
D = 1024
NT = S // 128
NE = 16
CAP = 1024
NJ = 5
CAPH = NJ * 128
DFF = 2048
EPS = 1e-6


class Sched:
    def __init__(self, nc, es):
        self.nc = nc
        self.es = es
        self.eng = {"pe": nc.tensor, "act": nc.scalar, "dve": nc.vector,
                    "pool": nc.gpsimd, "sp": nc.sync}
        self.sem = {k: es.enter_context(nc.semaphore("prog_" + k)) for k in self.eng}
        self.cnt = {k: 0 for k in self.eng}
        self.waited = {k: {} for k in self.eng}
        self.last_w = {}
        self.readers = {}
        self.dsem = {}

    def _wait(self, e, tok):
        name, sem, val = tok
        if name.startswith("d_"):
            val = max(val, self.dsem[name[2:]][1])
        if self.waited[e].get(name, 0) >= val:
            return
        self.eng[e].wait_ge(sem, val)
        self.waited[e][name] = val

    def _deps(self, e, reads, writes):
        toks = []
        for k in reads:
            if k in self.last_w:
                toks.append(self.last_w[k])
        for k in writes:
            rd = self.readers.get(k, [])
            if rd:
                toks += rd
            elif k in self.last_w:
                toks.append(self.last_w[k])
        best = {}
        for t in toks:
            if t[0] not in best or best[t[0]][2] < t[2]:
                best[t[0]] = t
        for t in best.values():
            if e == "pe" and t[0] == "pe":
                continue
            self._wait(e, t)

    def _record(self, tok, reads, writes):
        for k in reads:
            lst = self.readers.setdefault(k, [])
            lst[:] = [t for t in lst if t[0] != tok[0]]
            lst.append(tok)
        for k in writes:
            self.last_w[k] = tok
            self.readers[k] = []

    def op(self, e, fn, reads=(), writes=(), inc=True):
        self._deps(e, reads, writes)
        inst = fn(self.eng[e])
        if inc:
            self.cnt[e] += 1
            inst.then_inc(self.sem[e], 1)
            tok = (e, self.sem[e], self.cnt[e])
        else:
            tok = (e, self.sem[e], self.cnt[e] + 1)
        self._record(tok, reads, writes)
        return tok

    def dma(self, e, semname, fn, reads=(), writes=()):
        self._deps(e, reads, writes)
        if semname not in self.dsem:
            self.dsem[semname] = [self.es.enter_context(self.nc.semaphore("d_" + semname)), 0]
        s = self.dsem[semname]
        inst = fn(self.eng[e])
        s[1] += 16
        inst.then_inc(s[0], 16)
        tok = ("d_" + semname, s[0], s[1])
        self._record(tok, reads, writes)
        return tok

    def wait_all(self, e):
        for k in self.eng:
            if self.cnt[k] > 0:
                self._wait(e, (k, self.sem[k], self.cnt[k]))
        for name, s in self.dsem.items():
            if s[1] > 0:
                self._wait(e, ("d_" + name, s[0], s[1]))

    def barrier(self):
        snap = [(k, self.sem[k], self.cnt[k]) for k in self.eng if self.cnt[k] > 0]
        snap += [("d_" + n, s[0], s[1]) for n, s in self.dsem.items() if s[1] > 0]
        for e in self.eng:
            for t in snap:
                self._wait(e, t)
        self.last_w.clear()
        self.readers.clear()


def build_program(dbg=None):
    nc = bass.Bass("TRN2", target_bir_lowering=False)
    es = ExitStack()
    sc = Sched(nc, es)

    def din(name, shape, dt=F32):
        return nc.dram_tensor(name, list(shape), dt, kind="ExternalInput").ap()

    def dscr(name, shape, dt):
        return nc.dram_tensor(name, list(shape), dt, kind="Internal").ap()

    cur = [es]

    def sb(name, shape, dt):
        return cur[0].enter_context(nc.sbuf_tensor(name, list(shape), dt))

    def ps(name, shape, dt=F32):
        return cur[0].enter_context(nc.psum_tensor(name, list(shape), dt))

    x = din("x", [S, D])
    g1rep = din("g1rep", [128, D])
    w_in = din("w_in", [D, 1536])
    wfT = din("wfT", [128, 4, D])
    dft128 = din("dft128", [128, 256])
    ident_in = din("ident", [128, 128])
    g2rep = din("g2rep", [128, D])
    gfrep = din("gfrep", [128, D])
    gw_in = din("gw", [16, 128, 128])
    vecs_in = din("vecs", [512, 12])
    w2c_in = din("w2c", [128, 128 * 64], BF16)
    w2s_in = din("w2s", [128, 128 * 64], BF16)
    w_out = din("w_out", [D, D])
    w_router = din("w_router", [D, NE])
    w_gate = din("w_gate", [NE, D, DFF])
    w_up = din("w_up", [NE, D, DFF])
    w_down = din("w_down", [NE, DFF, D])
    bones_in = din("bones", [128, 128])
    mexcl_in = din("mexcl", [128, 128])
    blkoff_in = din("blkoff", [128, 1])
    halfmask_in = din("halfmask", [128, 1])
    dummyidx_in = din("dummyidx", [128, 1])
    out = nc.dram_tensor("out", [S, D], F32, kind="ExternalOutput").ap()

    Gd = dscr("Gd", [S, D], BF16)
    lxg = dscr("lxg", [1024, S], F32)
    lxb = dscr("lxb", [512, S], BF16)
    Zd = dscr("Zd", [4, 64, 128, 256], BF16)
    mixd = dscr("mixd", [1024, S], BF16)
    x2d = dscr("x2d", [S + 128, D], F32)
    h2d = dscr("h2d", [S + 128, D], BF16)
    scTd = dscr("scTd", [NE, S], F32)

    ident_f = sb("ident_f", [128, 128], F32)
    ident_b = sb("ident_b", [128, 128], BF16)
    g1 = sb("g1", [128, D], F32)
    eps_t = sb("eps_t", [128, 1], F32)
    sc.op("dve", lambda e: e.memset(eps_t[:], EPS), writes=["eps_t"])
    dft_f = sb("dft_f", [128, 256], F32)
    sc.dma("sp", "c4", lambda e: e.dma_start(out=dft_f[:], in_=dft128[:, :]), writes=["dft_f"])
    phW = ExitStack()
    cur[0] = phW
    W_all = sb("W_all", [128, 8, 2048], BF16)

    sc.dma("sp", "c0", lambda e: e.dma_start(out=ident_f[:], in_=ident_in[:, :]), writes=["ident_f"])
    sc.dma("sp", "c1", lambda e: e.dma_start(out=g1[:], in_=g1rep[:, :]), writes=["g1"])
    sc.op("dve", lambda e: e.tensor_copy(out=ident_b[:], in_=ident_f[:]), reads=["ident_f"], writes=["ident_b"])
    w_in_v = w_in.rearrange("(k p) n -> p k n", p=128)
    for k in range(8):
        sc.dma("pool", "c2", lambda e, k=k: e.dma_start(out=W_all[:, k, 0:1024], in_=w_in_v[:, k, 0:1024]),
               writes=[("W_all", k)])

    with nc.sbuf_tensor("wfT_sb", [128, 4, D], F32) as wfT_sb, \
            nc.psum_tensor("ps_w", [128, 1024], F32) as ps_w:
        sc.dma("sp", "c3", lambda e: e.dma_start(out=wfT_sb[:], in_=wfT[:, :, :]), writes=["wfT_sb"])
        for k in range(8):
            for g in range(4):
                sc.op("pe", lambda e, k=k, g=g: e.matmul(
                    ps_w[:, g * 256:(g + 1) * 256], lhsT=wfT_sb[:, g, k * 128:(k + 1) * 128],
                    rhs=dft_f[:, :], start=True, stop=True),
                    reads=["wfT_sb", "dft_f"], writes=[("ps_w", g)])
            psv = ps_w[:, :].rearrange("p (g c m) -> p g c m", g=4, c=2)
            sc.op("act", lambda e, k=k: e.activation(
                out=W_all[:, k, 1024:1536].rearrange("p (g m) -> p g m", g=4), in_=psv[:, :, 0, :], func=AF.Copy),
                reads=[("ps_w", g) for g in range(4)], writes=[("W_all", k)])
            sc.op("dve", lambda e, k=k: e.tensor_copy(
                out=W_all[:, k, 1536:2048].rearrange("p (g m) -> p g m", g=4), in_=psv[:, :, 1, :]),
                reads=[("ps_w", g) for g in range(4)], writes=[("W_all", k), ("ps_w", 0), ("ps_w", 1), ("ps_w", 2), ("ps_w", 3)])
        sc.barrier()

    phA = ExitStack()
    cur[0] = phA
    NB = 4
    xt = [sb(f"xt{i}", [128, D], F32) for i in range(NB)]
    hb = [sb(f"hb{i}", [128, D], BF16) for i in range(NB)]
    junk = sb("junk", [128, D], BF16)
    ss = [sb(f"ss{i}", [128, 1], F32) for i in range(NB)]
    sd = [sb(f"sd{i}", [128, 1], F32) for i in range(NB)]
    rs = [sb(f"rs{i}", [128, 1], F32) for i in range(NB)]
    hT = [sb(f"hT{i}", [128, 8, 512], BF16) for i in range(2)]
    gt = [sb(f"gt{i}", [128, D], BF16) for i in range(2)]
    lst = [sb(f"lst{i}", [128, 512], F32) for i in range(4)]
    lsb = [sb(f"lsb{i}", [128, 512], BF16) for i in range(4)]
    pt = [ps(f"pt{i}", [128, 8, 128], BF16) for i in range(2)]
    pf = [ps(f"pf{i}", [128, 1024], F32) for i in range(2)]
    pl = [ps(f"pl{i}", [128, 512], F32) for i in range(2)]

    def a_front(ti):
        b = ti % NB
        sc.dma("sp", f"xt{b}", lambda e: e.dma_start(out=xt[b][:], in_=x[ti * 128:(ti + 1) * 128, :]), writes=[("xt", b)])
        sc.op("act", lambda e: e.activation(out=junk[:], in_=xt[b][:], func=AF.Square, accum_out=ss[b][:]),
              reads=[("xt", b)], writes=["junk", ("ss", b)])
        sc.op("act", lambda e: e.activation(out=sd[b][:], in_=ss[b][:], func=AF.Ln, bias=eps_t[:, 0:1], scale=1.0 / D),
              reads=[("ss", b), "eps_t"], writes=[("sd", b)])
        sc.op("act", lambda e: e.activation(out=rs[b][:], in_=sd[b][:], func=AF.Exp, scale=-0.5), reads=[("sd", b)], writes=[("rs", b)])
        sc.op("dve", lambda e: e.scalar_tensor_tensor(
            out=hb[b][:], in0=xt[b][:], scalar=rs[b][:, 0:1], in1=g1[:], op0=ALU.mult, op1=ALU.mult),
            reads=[("xt", b), ("rs", b), "g1"], writes=[("hb", b)])

    def a_mid1(ti):
        b = ti % NB
        st, j = divmod(ti, 4)
        hs = st % 2
        pb = ti % 2
        for k in range(8):
            sc.op("pe", lambda e, k=k: e.transpose(pt[pb][:, k, :], hb[b][:, k * 128:(k + 1) * 128], ident_b[:]),
                  inc=(k == 7), reads=[("hb", b), "ident_b"], writes=[("pt", pb)])
        sc.op("act", lambda e: e.activation(out=hT[hs][:, :, j * 128:(j + 1) * 128], in_=pt[pb][:, :, :], func=AF.Copy),
              reads=[("pt", pb)], writes=[("hT", hs, j), ("pt", pb)])

    def a_mid2(ti):
        st, j = divmod(ti, 4)
        hs = st % 2
        pb = ti % 2
        for n in range(2):
            for k in range(8):
                sc.op("pe", lambda e, k=k, n=n: e.matmul(
                    pf[pb][:, n * 512:(n + 1) * 512], lhsT=hT[hs][:, k, j * 128:(j + 1) * 128],
                    rhs=W_all[:, k, 1024 + n * 512:1024 + (n + 1) * 512], start=(k == 0), stop=(k == 7)),
                    inc=(k == 7), reads=[("hT", hs, j), ("W_all", k)], writes=[("pf", pb, n)])

    def a_back(ti):
        pb = ti % 2
        sc.op("dve", lambda e: e.tensor_copy(out=gt[pb][:], in_=pf[pb][:, :]),
              reads=[("pf", pb, 0), ("pf", pb, 1)], writes=[("gt", pb), ("pf", pb, 0), ("pf", pb, 1)])
        sc.dma("pool", f"gt{pb}", lambda e: e.dma_start(out=Gd[ti * 128:(ti + 1) * 128, :], in_=gt[pb][:]),
               reads=[("gt", pb)], writes=[("Gd", ti)])

    lcount = [0]

    def a_lru(st):
        hs = st % 2
        for c in range(8):
            pb = c % 2
            for k in range(8):
                sc.op("pe", lambda e, k=k: e.matmul(
                    pl[pb][:, :], lhsT=W_all[:, k, c * 128:(c + 1) * 128], rhs=hT[hs][:, k, :],
                    start=(k == 0), stop=(k == 7)),
                    inc=(k == 7), reads=[("hT", hs, j) for j in range(4)] + [("W_all", k)], writes=[("pl", pb)])
            lb = lcount[0] % 4
            lcount[0] += 1
            if c < 4:
                if c % 2 == 0:
                    sc.op("act", lambda e: e.activation(out=lsb[lb][:], in_=pl[pb][:, :], func=AF.Copy),
                          reads=[("pl", pb)], writes=[("lst", lb), ("pl", pb)])
                else:
                    sc.op("dve", lambda e: e.tensor_copy(out=lsb[lb][:], in_=pl[pb][:, :]),
                          reads=[("pl", pb)], writes=[("lst", lb), ("pl", pb)])
                sc.dma("pool", f"lst{lb}", lambda e: e.dma_start(
                    out=lxb[c * 128:(c + 1) * 128, st * 512:(st + 1) * 512], in_=lsb[lb][:]),
                    reads=[("lst", lb)], writes=[("lxb", c, st)])
            else:
                sc.op("act", lambda e: e.activation(out=lst[lb][:], in_=pl[pb][:, :], func=AF.Gelu_apprx_tanh),
                      reads=[("pl", pb)], writes=[("lst", lb), ("pl", pb)])
                sc.dma("pool", f"lst{lb}", lambda e: e.dma_start(
                    out=lxg[c * 128:(c + 1) * 128, st * 512:(st + 1) * 512], in_=lst[lb][:]),
                    reads=[("lst", lb)], writes=[("lxg", c, st)])

    a_front(0)
    a_front(1)
    a_front(2)
    a_mid1(0)
    for ti in range(NT):
        if ti + 1 < NT:
            a_mid1(ti + 1)
        a_mid2(ti)
        if ti + 3 < NT:
            a_front(ti + 3)
        a_back(ti)
        if ti % 4 == 3:
            a_lru(ti // 4)
    sc.barrier()

    phA.close()
    phW.close()
    cur[0] = es

    def dbg_dump(items):
        toks = []
        for name, src, shape, dt in items:
            d = nc.dram_tensor(name, list(shape), dt, kind="ExternalOutput").ap()
            if len(shape) == 2:
                sc.dma("sp", "dbg", lambda e, d=d, src=src: e.dma_start(out=d[:, :], in_=src[:, :]))
            else:
                sc.dma("sp", "dbg", lambda e, d=d, src=src: e.dma_start(out=d, in_=src))
        sc.wait_all("sp")
        es.close()
        return nc

    if dbg == "A":
        return dbg_dump([("dbgG", Gd, [S, D], BF16), ("dbgL", lxg, [1024, S], F32)])

    ph = ExitStack()
    cur[0] = ph
    C128 = sb("C128", [128, 128], BF16)
    S128 = sb("S128", [128, 128], BF16)
    nS128 = sb("nS128", [128, 128], BF16)
    sc.op("dve", lambda e: e.tensor_copy(out=C128[:], in_=dft_f[:, 0:128]), reads=["dft_f"], writes=["C128"])
    sc.op("dve", lambda e: e.tensor_copy(out=S128[:], in_=dft_f[:, 128:256]), reads=["dft_f"], writes=["S128"])
    sc.op("act", lambda e: e.activation(out=nS128[:], in_=dft_f[:, 128:256], func=AF.Copy, scale=-1.0),
          reads=["dft_f"], writes=["nS128"])
    gin = [sb(f"gin{i}", [128, 8, D], BF16) for i in range(2)]
    zt = [sb(f"zt{i}", [128, 4, 2, 128], BF16) for i in range(4)]
    pz = [ps(f"pz{i}", [128, 1024], F32) for i in range(4)]
    Gv = Gd.rearrange("(p f) c -> p f c", f=64)
    for ch in range(8):
        gb = ch % 2
        sc.dma("sp", f"ld{gb}", lambda e, gb=gb, ch=ch: e.dma_start(out=gin[gb][:], in_=Gv[:, ch * 8:(ch + 1) * 8, :]),
               writes=[("gin", gb)])
        for s in range(8):
            sbi = ch * 8 + s
            zb = sbi % 4
            rd = [("gin", gb), "C128", "S128", "nS128"]
            sc.op("pe", lambda e, zb=zb, gb=gb, s=s: e.matmul(pz[zb][:, 0:512], lhsT=C128[:], rhs=gin[gb][:, s, 0:512], start=True, stop=False), inc=False, reads=rd, writes=[("pz", zb, 0)])
            sc.op("pe", lambda e, zb=zb, gb=gb, s=s: e.matmul(pz[zb][:, 0:512], lhsT=nS128[:], rhs=gin[gb][:, s, 512:1024], start=False, stop=True), reads=rd, writes=[("pz", zb, 0)])
            sc.op("pe", lambda e, zb=zb, gb=gb, s=s: e.matmul(pz[zb][:, 512:1024], lhsT=S128[:], rhs=gin[gb][:, s, 0:512], start=True, stop=False), inc=False, reads=rd, writes=[("pz", zb, 1)])
            sc.op("pe", lambda e, zb=zb, gb=gb, s=s: e.matmul(pz[zb][:, 512:1024], lhsT=C128[:], rhs=gin[gb][:, s, 512:1024], start=False, stop=True), reads=rd, writes=[("pz", zb, 1)])
            sc.op("act", lambda e, zb=zb: e.activation(out=zt[zb][:, :, 0, :], in_=pz[zb][:, 0:512].rearrange("p (g m) -> p g m", g=4), func=AF.Copy),
                  reads=[("pz", zb, 0)], writes=[("zt", zb, 0), ("pz", zb, 0)])
            sc.op("dve", lambda e, zb=zb: e.tensor_copy(out=zt[zb][:, :, 1, :], in_=pz[zb][:, 512:1024].rearrange("p (g m) -> p g m", g=4)),
                  reads=[("pz", zb, 1)], writes=[("zt", zb, 1), ("pz", zb, 1)])
            sc.dma("pool", f"st{zb}", lambda e, zb=zb, sbi=sbi: e.dma_start(
                out=Zd[:, sbi, :, :].rearrange("g k c -> k g c"), in_=zt[zb][:, :, :, :].rearrange("p g r m -> p g (r m)")),
                reads=[("zt", zb, 0), ("zt", zb, 1)], writes=[("Zd", sbi)])
    sc.barrier()
    ph.close()
    cur[0] = es

    ph = ExitStack()
    cur[0] = ph
    gw_f = sb("gw_f", [128, 16, 128], F32)
    gw_b = sb("gw_b", [128, 16, 128], BF16)
    vecs = sb("vecs_sb", [128, 4, 12], F32)
    L8 = sb("L8", [128, 4, 2], F32)
    ltmp = sb("ltmp", [128, 4, 2], F32)
    sc.dma("sp", "c0", lambda e: e.dma_start(out=gw_f[:], in_=gw_in.rearrange("m i j -> i m j")), writes=["gw_f"])
    sc.dma("sp", "c1", lambda e: e.dma_start(out=vecs[:], in_=vecs_in.rearrange("(c p) v -> p c v", p=128)), writes=["vecs"])
    sc.op("dve", lambda e: e.tensor_copy(out=gw_b[:], in_=gw_f[:]), reads=["gw_f"], writes=["gw_b"])
    for d, col in ((0, 7), (1, 10)):
        sc.op("act", lambda e, d=d, col=col: e.activation(out=ltmp[:, :, d], in_=vecs[:, :, col], func=AF.Exp, scale=-1.0),
              reads=["vecs"], writes=["ltmp"])
        sc.op("act", lambda e, d=d: e.activation(out=ltmp[:, :, d], in_=ltmp[:, :, d], func=AF.Ln, bias=1.0, scale=1.0),
              reads=["ltmp"], writes=["ltmp"])
        sc.op("dve", lambda e, d=d: e.tensor_scalar(out=L8[:, :, d], in0=ltmp[:, :, d], scalar1=-8.0, scalar2=None, op0=ALU.mult),
              reads=["ltmp"], writes=["L8"])
    Xbf = sb("Xbf", [128, S + 4], BF16)
    BH = sb("BH", [128, S], F32)
    B2 = sb("B2", [128, S], F32)
    B3 = sb("B3", [128, S], F32)
    B4 = sb("B4", [128, S], F32)
    cbf = sb("cbf", [128, S], BF16)
    ybf = sb("ybf", [128, S], BF16)
    dg = sb("dg", [128, 16, 128], BF16)
    pgA = [ps(f"pgA{i}", [128, 1024], F32) for i in range(2)]
    pgX = [ps(f"pgX{i}", [128, 1024], F32) for i in range(2)]
    sc.op("dve", lambda e: e.memset(Xbf[:, 0:2], 0.0), writes=["Xpad"])
    sc.op("dve", lambda e: e.memset(Xbf[:, S + 2:S + 4], 0.0), writes=["Xpad"])
    for cc in range(4):
        for k in range(4):
            sc.op("dve", lambda e, cc=cc, k=k: e.tensor_scalar(out=dg[:, cc * 4 + k, :], in0=ident_f[:], scalar1=vecs[:, cc, k:k + 1], scalar2=None, op0=ALU.mult),
                  reads=["ident_f", "vecs"], writes=["dg"])
    PIECE = 2048
    NP = S // PIECE
    HALF = S // 2

    def load_x(cc):
        for hh in range(2):
            sc.dma("sp", f"ld{hh}", lambda e, hh=hh: e.dma_start(
                out=Xbf[:, 2 + hh * HALF:2 + (hh + 1) * HALF], in_=lxb[cc * 128:(cc + 1) * 128, hh * HALF:(hh + 1) * HALF]),
                writes=[("X", hh)])

    def conv(cc):
        for q in range(S // 1024):
            o2 = q * 1024
            pa = q % 2
            p = o2 // PIECE
            for hf_ in range(2):
                for k in range(4):
                    sc.op("pe", lambda e, o2=o2, pa=pa, hf_=hf_, k=k: e.matmul(
                        pgA[pa][:, hf_ * 512:(hf_ + 1) * 512], lhsT=dg[:, cc * 4 + k, :], rhs=Xbf[:, o2 + hf_ * 512 + k:o2 + hf_ * 512 + k + 512],
                        start=(k == 0), stop=(k == 3)), inc=(k == 3 and hf_ == 1), reads=["dg", ("X", 0), ("X", 1), "Xpad"], writes=[("pgA", pa)])
            sc.op("act", lambda e, o2=o2, pa=pa: e.activation(out=cbf[:, o2:o2 + 1024], in_=pgA[pa][:, :], func=AF.Identity, bias=vecs[:, cc, 4:5], scale=1.0),
                  reads=[("pgA", pa), "vecs"], writes=[("cbf", p), ("pgA", pa)])
        if cc + 1 < 4:
            load_x(cc + 1)

    load_x(0)
    for cc in range(4):
        if cc == 0:
            conv(0)
        for d in range(2):
            ba_col = 5 if d == 0 else 8
            bx_col = 6 if d == 0 else 9
            order = list(range(NP)) if d == 0 else list(range(NP - 1, -1, -1))
            for p in range(NP):
                for tq in range(PIECE // 1024):
                    o2 = p * PIECE + tq * 1024
                    pa = tq % 2
                    for hf_ in range(2):
                        sl = slice(o2 + hf_ * 512, o2 + (hf_ + 1) * 512)
                        sc.op("pe", lambda e, sl=sl, pa=pa, hf_=hf_: e.matmul(pgA[pa][:, hf_ * 512:(hf_ + 1) * 512], lhsT=gw_b[:, (2 * d) * 4 + cc, :], rhs=cbf[:, sl], start=True, stop=True),
                              inc=(hf_ == 1), reads=["gw_b", ("cbf", p)], writes=[("pgA", pa)])
                    sc.op("act", lambda e, o2=o2, pa=pa: e.activation(out=B2[:, o2:o2 + 1024], in_=pgA[pa][:, :], func=AF.Sigmoid, bias=vecs[:, cc, ba_col:ba_col + 1], scale=1.0),
                          reads=[("pgA", pa), "vecs"], writes=[("B2", p), ("pgA", pa)])
                    for hf_ in range(2):
                        sl = slice(o2 + hf_ * 512, o2 + (hf_ + 1) * 512)
                        sc.op("pe", lambda e, sl=sl, pa=pa, hf_=hf_: e.matmul(pgX[pa][:, hf_ * 512:(hf_ + 1) * 512], lhsT=gw_b[:, (2 * d + 1) * 4 + cc, :], rhs=cbf[:, sl], start=True, stop=True),
                              inc=(hf_ == 1), reads=["gw_b", ("cbf", p)], writes=[("pgX", pa)])
                    sc.op("act", lambda e, o2=o2, pa=pa: e.activation(out=B3[:, o2:o2 + 1024], in_=pgX[pa][:, :], func=AF.Sigmoid, bias=vecs[:, cc, bx_col:bx_col + 1], scale=1.0),
                          reads=[("pgX", pa), "vecs"], writes=[("B3", p), ("pgX", pa)])

            def exp_a(p):
                psl = slice(p * PIECE, (p + 1) * PIECE)
                sc.op("act", lambda e: e.activation(out=B2[:, psl], in_=B2[:, psl], func=AF.Exp, scale=L8[:, cc, d:d + 1]),
                      reads=[("B2", p), "L8"], writes=[("B2", p)])
                if p % 2 == 0:
                    sc.op("act", lambda e: e.activation(out=B4[:, psl], in_=B2[:, psl], func=AF.Square), reads=[("B2", p)], writes=[("B4", p)])
                else:
                    sc.op("dve", lambda e: e.tensor_tensor(out=B4[:, psl], in0=B2[:, psl], in1=B2[:, psl], op=ALU.mult), reads=[("B2", p)], writes=[("B4", p)])
                sc.op("dve", lambda e: e.tensor_tensor(out=B3[:, psl], in0=B3[:, psl], in1=cbf[:, psl], op=ALU.mult),
                      reads=[("B3", p), ("cbf", p)], writes=[("B3", p)])

            exp_a(order[0])
            for pi, p in enumerate(order):
                o = p * PIECE
                psl = slice(o, o + PIECE)
                k2, k3, k4, kh = ("B2", p), ("B3", p), ("B4", p), ("BH", p)
                if pi + 1 < NP:
                    exp_a(order[pi + 1])
                sc.op("act", lambda e, psl=psl: e.activation(out=B4[:, psl], in_=B4[:, psl], func=AF.Ln, bias=1.0, scale=-1.0), reads=[k4], writes=[k4])
                sc.op("act", lambda e, psl=psl: e.activation(out=B4[:, psl], in_=B4[:, psl], func=AF.Exp, scale=0.5), reads=[k4], writes=[k4])
                sc.op("dve", lambda e, psl=psl: e.tensor_tensor(out=B3[:, psl], in0=B3[:, psl], in1=B4[:, psl], op=ALU.mult), reads=[k3, k4], writes=[k3])
                if d == 0:
                    init = 0.0 if pi == 0 else BH[:, o - 1:o]
                    rd = [k2, k3] + ([("BH", p - 1)] if pi > 0 else [])
                    sc.op("dve", lambda e, psl=psl, init=init: e.tensor_tensor_scan(
                        out=BH[:, psl], data0=B2[:, psl], data1=B3[:, psl], initial=init, op0=ALU.mult, op1=ALU.add),
                        reads=rd, writes=[kh])
                else:
                    init = 0.0 if pi == 0 else B4[:, o + PIECE:o + PIECE + 1]
                    rd = [k2, k3, k4] + ([("B4", p + 1)] if pi > 0 else [])
                    sc.op("dve", lambda e, init=init, o=o: e.tensor_tensor_scan(
                        out=B4[:, o:o + PIECE][:, ::-1], data0=B2[:, o:o + PIECE][:, ::-1], data1=B3[:, o:o + PIECE][:, ::-1], initial=init, op0=ALU.mult, op1=ALU.add),
                        reads=rd, writes=[k4])
                    sc.dma("sp", f"ld{2 + (p % 2)}", lambda e, o=o, psl=psl: e.dma_start(out=B2[:, psl], in_=lxg[512 + cc * 128:512 + (cc + 1) * 128, o:o + PIECE]),
                           reads=[], writes=[k2])
        if cc + 1 < 4:
            conv(cc + 1)
        for p in range(NP):
            psl = slice(p * PIECE, (p + 1) * PIECE)
            sc.op("dve", lambda e, psl=psl: e.tensor_tensor(out=B4[:, psl], in0=B4[:, psl], in1=BH[:, psl], op=ALU.add),
                  reads=[("B4", p), ("BH", p)], writes=[("B4", p)])
            sc.op("dve", lambda e, psl=psl: e.tensor_tensor(out=ybf[:, psl], in0=B4[:, psl], in1=B2[:, psl], op=ALU.mult),
                  reads=[("B4", p), ("B2", p)], writes=[("ybf", p)])
        sc.dma("sp", "st0", lambda e, cc=cc: e.dma_start(out=mixd[cc * 128:(cc + 1) * 128, :], in_=ybf[:]), reads=[("ybf", p) for p in range(NP)], writes=[("mixd", cc)])
    sc.barrier()
    ph.close()
    cur[0] = es

    if dbg == "L":
        return dbg_dump([("dbgM", mixd, [1024, S], BF16)])

    g2 = sb("g2", [128, D], F32)
    sc.dma("sp", "c0", lambda e: e.dma_start(out=g2[:], in_=g2rep[:, :]), writes=["g2"])
    scores_tm = sb("scores_tm", [128, NT, NE], F32)
    phO_pre = ExitStack()
    cur[0] = phO_pre
    Wo = sb("Wo", [128, 8, D], BF16)
    Wr = sb("Wr", [128, 8, NE], BF16)
    wo_v = w_out.rearrange("(k p) n -> p k n", p=128)
    for k in range(8):
        sc.dma("pool", "c2", lambda e, k=k: e.dma_start(out=Wo[:, k, :], in_=wo_v[:, k, :]), writes=[("Wo", k)])
    sc.dma("pool", "c2", lambda e: e.dma_start(out=Wr[:], in_=w_router.rearrange("(k p) n -> p k n", p=128)), writes=["Wr"])
    ph = ExitStack()
    cur[0] = ph
    W2c = sb("W2c", [128, 128, 64], BF16)
    W2s = sb("W2s", [128, 128, 64], BF16)
    sc.dma("act", "c0", lambda e: e.dma_start(out=W2c[:].rearrange("p a b -> p (a b)"), in_=w2c_in[:, :]), writes=["W2c"])
    sc.dma("act", "c1", lambda e: e.dma_start(out=W2s[:].rearrange("p a b -> p (a b)"), in_=w2s_in[:, :]), writes=["W2s"])
    NZ = 3
    Zh = [sb(f"Zh{i}", [128, 64, 256], BF16) for i in range(NZ)]
    yfg = [sb(f"yfg{i}", [128, S], BF16) for i in range(2)]
    py = [ps(f"py{i}", [128, 512], F32) for i in range(2)]
    cnt = 0
    for g in range(4):
        yb = g % 2
        zb = g % NZ
        for hf in range(2):
            sc.dma("sp", f"ld{zb}", lambda e, zb=zb, g=g, hf=hf: e.dma_start(out=Zh[zb][hf * 64:(hf + 1) * 64, :, :], in_=Zd[g, :, hf * 64:(hf + 1) * 64, :]),
                   writes=[("Zh", zb, hf)])
        for hf in range(2):
            r0 = hf * 64
            for a8 in range(8):
                pb = cnt % 2
                cnt += 1
                for a in range(8):
                    kl = a8 * 8 + a
                    ka = hf * 64 + kl
                    sc.op("pe", lambda e, zb=zb, kl=kl, ka=ka, a=a, pb=pb, r0=r0: e.matmul(py[pb][:, a * 64:(a + 1) * 64], lhsT=Zh[zb][r0:r0 + 64, kl, 0:128], rhs=W2c[r0:r0 + 64, ka, :], start=True, stop=False),
                          inc=False, reads=[("Zh", zb, hf), "W2c"], writes=[("py", pb)])
                    sc.op("pe", lambda e, zb=zb, kl=kl, ka=ka, a=a, pb=pb, r0=r0: e.matmul(py[pb][:, a * 64:(a + 1) * 64], lhsT=Zh[zb][r0:r0 + 64, kl, 128:256], rhs=W2s[r0:r0 + 64, ka, :], start=False, stop=True),
                          inc=(a == 7), reads=[("Zh", zb, hf), "W2s"], writes=[("py", pb)])
                a0 = hf * 64 + a8 * 8
                ov = yfg[yb][:, :].rearrange("p (b a) -> p b a", a=128)[:, :, a0:a0 + 8]
                iv = py[pb][:, :].rearrange("p (a b) -> p b a", a=8)
                if cnt % 2 == 0:
                    sc.op("act", lambda e, ov=ov, iv=iv: e.activation(out=ov, in_=iv, func=AF.Copy), reads=[("py", pb)], writes=[("yfg", yb), ("py", pb)])
                else:
                    sc.op("dve", lambda e, ov=ov, iv=iv: e.tensor_copy(out=ov, in_=iv), reads=[("py", pb)], writes=[("yfg", yb), ("py", pb)])
        sc.dma("pool", f"st{yb}", lambda e, yb=yb, g=g: e.dma_start(out=mixd[512 + g * 128:512 + (g + 1) * 128, :], in_=yfg[yb][:]),
               reads=[("yfg", yb)], writes=[("mixd", 4 + g)])
    sc.barrier()
    ph.close()
    cur[0] = es

    if dbg == "F":
        return dbg_dump([("dbgM", mixd, [1024, S], BF16)])

    ph = ExitStack()
    cur[0] = ph
    mT = [sb(f"mT{i}", [128, 8, 512], BF16) for i in range(2)]
    NO = 4
    xt = [sb(f"oxt{i}", [128, D], F32) for i in range(NO)]
    x2t = [sb(f"x2t{i}", [128, D], F32) for i in range(NO)]
    h2b = [sb(f"h2b{i}", [128, D], BF16) for i in range(NO)]
    h2T = [sb(f"h2T{i}", [128, 8, 128], BF16) for i in range(2)]
    junk2 = sb("junk2", [128, D], BF16)
    st_ss = [sb(f"oss{i}", [128, 1], F32) for i in range(NO)]
    st_sd = [sb(f"osd{i}", [128, 1], F32) for i in range(NO)]
    st_rs = [sb(f"ors{i}", [128, 1], F32) for i in range(NO)]
    st_mx = [sb(f"omx{i}", [128, 1], F32) for i in range(2)]
    st_se = [sb(f"ose{i}", [128, 1], F32) for i in range(2)]
    st_ex = [sb(f"oex{i}", [128, NE], F32) for i in range(2)]
    po = [ps(f"po{i}", [128, D], F32) for i in range(2)]
    pt2 = [ps(f"pt2{i}", [128, 8, 128], BF16) for i in range(2)]
    plog = [ps(f"plog{i}", [128, NE], F32) for i in range(2)]
    mixv = mixd.rearrange("(k p) t -> p k t", p=128)

    def o_a(ti):
        st, j = divmod(ti, 4)
        ms = st % 2
        b = ti % NO
        pb = ti % 2
        if j == 0:
            sc.dma("sp", f"ld{ms}", lambda e: e.dma_start(out=mT[ms][:], in_=mixv[:, :, st * 512:(st + 1) * 512]), writes=[("mT", ms)])
        sc.dma("sp", f"xt{b}", lambda e: e.dma_start(out=xt[b][:], in_=x[ti * 128:(ti + 1) * 128, :]), writes=[("oxt", b)])
        for n in range(2):
            for k in range(8):
                sc.op("pe", lambda e, k=k, n=n: e.matmul(po[pb][:, n * 512:(n + 1) * 512], lhsT=mT[ms][:, k, j * 128:(j + 1) * 128],
                                                       rhs=Wo[:, k, n * 512:(n + 1) * 512], start=(k == 0), stop=(k == 7)),
                      inc=(k == 7), reads=[("mT", ms), ("Wo", k)], writes=[("po", pb, n)])
        sc.op("dve", lambda e: e.tensor_tensor(out=x2t[b][:], in0=po[pb][:, :], in1=xt[b][:], op=ALU.add),
              reads=[("po", pb, 0), ("po", pb, 1), ("oxt", b)], writes=[("x2t", b), ("po", pb, 0), ("po", pb, 1)])
        sc.dma("pool", f"st{b}", lambda e: e.dma_start(out=x2d[ti * 128:(ti + 1) * 128, :], in_=x2t[b][:]), reads=[("x2t", b)], writes=[("x2d", ti)])
        sc.op("act", lambda e: e.activation(out=junk2[:], in_=x2t[b][:], func=AF.Square, accum_out=st_ss[b][:]), reads=[("x2t", b)], writes=["junk2", ("oss", b)])
        sc.op("act", lambda e: e.activation(out=st_sd[b][:], in_=st_ss[b][:], func=AF.Ln, bias=eps_t[:, 0:1], scale=1.0 / D), reads=[("oss", b), "eps_t"], writes=[("osd", b)])
        sc.op("act", lambda e: e.activation(out=st_rs[b][:], in_=st_sd[b][:], func=AF.Exp, scale=-0.5), reads=[("osd", b)], writes=[("ors", b)])
        sc.op("dve", lambda e: e.scalar_tensor_tensor(out=h2b[b][:], in0=x2t[b][:], scalar=st_rs[b][:, 0:1], in1=g2[:], op0=ALU.mult, op1=ALU.mult),
              reads=[("x2t", b), ("ors", b), "g2"], writes=[("h2b", b)])
        sc.dma("pool", f"h2{b}", lambda e: e.dma_start(out=h2d[ti * 128:(ti + 1) * 128, :], in_=h2b[b][:]), reads=[("h2b", b)], writes=[("h2d", ti)])

    def o_b1(ti):
        b = ti % NO
        pb = ti % 2
        for k in range(8):
            sc.op("pe", lambda e, k=k: e.transpose(pt2[pb][:, k, :], h2b[b][:, k * 128:(k + 1) * 128], ident_b[:]), inc=(k == 7), reads=[("h2b", b), "ident_b"], writes=[("pt2", pb)])
        sc.op("act", lambda e: e.activation(out=h2T[pb][:], in_=pt2[pb][:, :, :], func=AF.Copy), reads=[("pt2", pb)], writes=[("h2T", pb), ("pt2", pb)])

    def o_b2(ti):
        pb = ti % 2
        for k in range(8):
            sc.op("pe", lambda e, k=k: e.matmul(plog[pb][:, :], lhsT=h2T[pb][:, k, :], rhs=Wr[:, k, :], start=(k == 0), stop=(k == 7)),
                  inc=(k == 7), reads=[("h2T", pb), "Wr"], writes=[("plog", pb)])
        sc.op("dve", lambda e: e.tensor_reduce(out=st_mx[pb][:], in_=plog[pb][:, :], axis=AX.X, op=ALU.max, negate=True), reads=[("plog", pb)], writes=[("omx", pb)])
        sc.op("act", lambda e: e.activation(out=st_ex[pb][:], in_=plog[pb][:, :], func=AF.Exp, bias=st_mx[pb][:, 0:1], scale=1.0, accum_out=st_se[pb][:]),
              reads=[("plog", pb), ("omx", pb)], writes=[("oex", pb), ("ose", pb), ("plog", pb)])
        sc.op("dve", lambda e: e.reciprocal(out=st_se[pb][:], in_=st_se[pb][:]), reads=[("ose", pb)], writes=[("ose", pb)])
        sc.op("dve", lambda e: e.tensor_scalar(out=scores_tm[:, ti, :], in0=st_ex[pb][:], scalar1=st_se[pb][:, 0:1], scalar2=None, op0=ALU.mult),
              reads=[("oex", pb), ("ose", pb)], writes=[("scores", ti)])

    o_a(0)
    o_a(1)
    o_a(2)
    o_b1(0)
    for ti in range(NT):
        if ti + 1 < NT:
            o_b1(ti + 1)
        o_b2(ti)
        if ti + 3 < NT:
            o_a(ti + 3)
    sc.barrier()
    ph.close()
    cur[0] = es

    phO_pre.close()
    cur[0] = es

    if dbg == "O":
        return dbg_dump([("dbgX2", x2d, [S, D], F32), ("dbgH2", h2d, [S, D], BF16)])

    idxT = sb("idxT", [128, 8, NE], U32)
    gateT = sb("gateT", [128, 8, NE], F32)
    NRING = 6
    ring = [sb(f"wr{i}", [128, 8, 1024], BF16) for i in range(NRING)]
    wg_v = w_gate.rearrange("e (k p) f -> e p k f", p=128)
    wu_v = w_up.rearrange("e (k p) f -> e p k f", p=128)
    wd_v = w_down.rearrange("e (k p) d -> e p k d", p=128)

    def load_chunk(slot, src_ap):
        for k in range(8):
            sc.dma("pool", f"wr{slot}", lambda e, slot=slot, k=k: e.dma_start(out=ring[slot][:, k, :], in_=src_ap[:, k, :]), writes=[("ring", slot, k)])

    def issue_weights_gu(e_, h):
        load_chunk(2 * h, wg_v[e_, :, :, h * 1024:(h + 1) * 1024])
        load_chunk(2 * h + 1, wu_v[e_, :, :, h * 1024:(h + 1) * 1024])

    def issue_weights_d(e_):
        for h in range(2):
            load_chunk(4 + h, wd_v[e_, :, h * 8:(h + 1) * 8, :])

    issue_weights_gu(0, 0)
    issue_weights_gu(0, 1)
    issue_weights_d(0)
    ph = ExitStack()
    cur[0] = ph
    scT = sb("scT", [NE, S], F32)
    pT = [ps(f"pT{i}", [128, 512], F32) for i in range(2)]
    for q in range(NT // 4):
        pb = q % 2
        for i in range(4):
            ti = q * 4 + i
            sc.op("pe", lambda e, pb=pb, i=i, ti=ti: e.transpose(pT[pb][0:NE, i * 128:(i + 1) * 128], scores_tm[:, ti, :], ident_f[:]),
                  inc=(i == 3), reads=["ident_f"], writes=[("pT", pb)])
        sc.op("act" if q % 2 == 0 else "dve",
              (lambda e, pb=pb, q=q: e.activation(out=scT[:, q * 512:(q + 1) * 512], in_=pT[pb][0:NE, :], func=AF.Copy)) if q % 2 == 0 else
              (lambda e, pb=pb, q=q: e.tensor_copy(out=scT[:, q * 512:(q + 1) * 512], in_=pT[pb][0:NE, :])),
              reads=[("pT", pb)], writes=["scT", ("pT", pb)])
    sc.dma("sp", "st0", lambda e: e.dma_start(out=scTd[:, :], in_=scT[:]), reads=["scT"], writes=["scTd"])
    sc128 = sb("sc128", [128, 1024], F32)
    work = sb("work", [128, 1024], F32)
    sc.dma("sp", "ld0", lambda e: e.dma_start(out=sc128[:], in_=scTd.rearrange("e (b t) -> (e b) t", b=8)), reads=["scTd"], writes=["sc128"])
    bones = sb("bones_sb", [128, 128], F32)
    mexcl = sb("mexcl_sb", [128, 128], F32)
    blkoff = sb("blkoff_sb", [128, 1], F32)
    sc.dma("sp", "c0", lambda e: e.dma_start(out=bones[:], in_=bones_in[:, :]), writes=["bones"])
    sc.dma("sp", "c1", lambda e: e.dma_start(out=mexcl[:], in_=mexcl_in[:, :]), writes=["mexcl"])
    sc.dma("sp", "c3", lambda e: e.dma_start(out=blkoff[:], in_=blkoff_in[:, :]), writes=["blkoff"])
    halfm = sb("halfm", [128, 1], F32)
    dummyi = sb("dummyi", [128, 1], F32)
    sc.dma("sp", "c4", lambda e: e.dma_start(out=halfm[:], in_=halfmask_in[:, :]), writes=["halfm"])
    sc.dma("sp", "c4", lambda e: e.dma_start(out=dummyi[:], in_=dummyidx_in[:, :]), writes=["dummyi"])
    zrow = sb("zrow", [128, D], F32)
    zrowb = sb("zrowb", [128, D], BF16)
    sc.op("dve", lambda e: e.memset(zrow[:], 0.0), writes=["zrow"])
    sc.op("dve", lambda e: e.memset(zrowb[:], 0.0), writes=["zrowb"])
    sc.dma("sp", "c4", lambda e: e.dma_start(out=x2d[S:S + 128, :], in_=zrow[:]), reads=["zrow"], writes=["x2dummy"])
    sc.dma("sp", "c4", lambda e: e.dma_start(out=h2d[S:S + 128, :], in_=zrowb[:]), reads=["zrowb"], writes=["h2dummy"])
    lo = sb("lo", [128, 1], F32)
    hi = sb("hi", [128, 1], F32)
    mid = sb("mid", [128, 1], F32)
    cntt = sb("cntt", [128, 2], F32)
    mge = sb("mge", [128, 1], U32)
    mlt = sb("mlt", [128, 1], U32)
    ptot = ps("ptot", [128, 2], F32)
    sc.op("dve", lambda e: e.memset(cntt[:], 0.0), writes=["cntt"])
    sc.op("dve", lambda e: e.memset(lo[:], 0.0), writes=["lo"])
    sc.op("dve", lambda e: e.memset(hi[:], 1.0), writes=["hi"])
    NR = 32
    vals = sb("vals", [128, NR * 8], F32)
    idxu = sb("idxu", [128, NR * 8], U32)
    cjunk = sb("cjunk", [128, 1024], F32)
    sc.op("act", lambda e: e.activation(out=work[:], in_=sc128[:], func=AF.Copy), reads=["sc128"], writes=["work"])
    NBIS = 30

    def bis1():
        sc.op("dve", lambda e: e.tensor_scalar(out=mid[:], in0=lo[:], scalar1=hi[:, 0:1], scalar2=0.5, op0=ALU.add, op1=ALU.mult), reads=["lo", "hi"], writes=["mid"])
        sc.op("dve", lambda e: e.tensor_scalar(out=cjunk[:], in0=sc128[:], scalar1=mid[:, 0:1], scalar2=None, op0=ALU.is_ge, op1=ALU.add, accum_out=cntt[:, 0:1]),
              reads=["sc128", "mid"], writes=["cjunk", "cntt"])
        sc.op("pe", lambda e: e.matmul(ptot[:, :], lhsT=bones[:], rhs=cntt[:, :], start=True, stop=True), reads=["bones", "cntt"], writes=["ptot"])

    def bis2():
        sc.op("dve", lambda e: e.tensor_scalar(out=mge[:], in0=ptot[:, 0:1], scalar1=float(CAP), scalar2=None, op0=ALU.is_ge), reads=["ptot"], writes=["mge"])
        sc.op("dve", lambda e: e.tensor_scalar(out=mlt[:], in0=ptot[:, 0:1], scalar1=float(CAP), scalar2=None, op0=ALU.is_lt), reads=["ptot"], writes=["mlt", "ptot"])
        sc.op("dve", lambda e: e.copy_predicated(out=lo[:], mask=mge[:], data=mid[:]), reads=["mge", "mid", "lo"], writes=["lo"])
        sc.op("dve", lambda e: e.copy_predicated(out=hi[:], mask=mlt[:], data=mid[:]), reads=["mlt", "mid", "hi"], writes=["hi"])

    for r in range(max(NR, NBIS)):
        sl = slice(r * 8, (r + 1) * 8)
        if r < NBIS:
            bis1()
        if r < NR:
            sc.op("dve", lambda e, sl=sl: e.max(out=vals[:, sl], in_=work[:]), reads=["work"], writes=[("vals", r)])
            sc.op("dve", lambda e, sl=sl: e.max_index(out=idxu[:, sl], in_max=vals[:, sl], in_values=work[:]), reads=["work", ("vals", r)], writes=[("idxu", r)])
        if r < NBIS:
            bis2()
        if r < NR:
            sc.op("dve", lambda e, sl=sl: e.match_replace(out=work[:], in_to_replace=vals[:, sl], in_values=work[:], imm_value=-1.0), reads=["work", ("vals", r)], writes=["work"])
    allv = [("vals", r) for r in range(NR)]
    alli = [("idxu", r) for r in range(NR)]
    W = CAP + NR * 8
    A = [sb(f"Ash{i}", [128, 2, W], F32) for i in range(2)]
    vm = sb("vm", [128, NR * 8], F32)
    idxf = sb("idxf", [128, NR * 8], F32)
    cnt2 = sb("cnt2", [128, 2], F32)
    Sf = sb("Sf", [128, 1], F32)
    Si = sb("Si", [128, 1], I32)
    bti = sb("bti", [128, 10], I32)
    btf = sb("btf", [128, 10], F32)
    nbf = sb("nbf", [128, 10], F32)
    sc.op("pool", lambda e: e.memset(A[0][:], 0.0), writes=["A0"])
    sc.op("pool", lambda e: e.memset(A[1][:], 0.0), writes=["A1"])
    sc.op("dve", lambda e: e.memset(cnt2[:], 0.0), writes=["cnt2"])
    sc.op("dve", lambda e: e.tensor_scalar(out=vm[:], in0=vals[:], scalar1=lo[:, 0:1], scalar2=None, op0=ALU.is_ge), reads=allv + ["lo"], writes=["vm"])
    sc.op("dve", lambda e: e.tensor_scalar(out=vm[:], in0=vm[:], scalar1=halfm[:, 0:1], scalar2=None, op0=ALU.mult, op1=ALU.add, accum_out=cnt2[:, 0:1]),
          reads=["vm", "halfm"], writes=["vm", "cnt2"])
    sc.op("dve", lambda e: e.tensor_copy(out=idxf[:], in_=idxu[:]), reads=alli, writes=["idxf"])
    sc.op("dve", lambda e: e.tensor_tensor(out=A[0][:, 0, 0:NR * 8], in0=vals[:], in1=vm[:], op=ALU.mult), reads=allv + ["vm", "A0"], writes=["A0"])
    sc.op("dve", lambda e: e.scalar_tensor_tensor(out=A[0][:, 1, 0:NR * 8], in0=idxf[:], scalar=blkoff[:, 0:1], in1=vm[:], op0=ALU.add, op1=ALU.mult),
          reads=["idxf", "blkoff", "vm", "A0"], writes=["A0"])
    sc.op("pe", lambda e: e.matmul(ptot[:, :], lhsT=mexcl[:], rhs=cnt2[:, :], start=True, stop=True), reads=["mexcl", "cnt2"], writes=["ptot"])
    sc.op("dve", lambda e: e.tensor_copy(out=Sf[:], in_=ptot[:, 0:1]), reads=["ptot"], writes=["Sf"])
    sc.op("dve", lambda e: e.tensor_copy(out=Si[:], in_=Sf[:]), reads=["Sf"], writes=["Si"])
    for jb in range(10):
        sc.op("dve", lambda e, jb=jb: e.tensor_scalar(out=bti[:, jb:jb + 1], in0=Si[:], scalar1=jb, scalar2=1, op0=ALU.logical_shift_right, op1=ALU.bitwise_and),
              reads=["Si"], writes=["bti"])
    sc.op("dve", lambda e: e.tensor_copy(out=btf[:], in_=bti[:]), reads=["bti"], writes=["btf"])
    sc.op("dve", lambda e: e.tensor_scalar(out=nbf[:], in0=btf[:], scalar1=-1.0, scalar2=1.0, op0=ALU.mult, op1=ALU.add), reads=["btf"], writes=["nbf"])
    for jb in range(10):
        sh = 1 << jb
        a, bq = A[jb % 2], A[(jb + 1) % 2]
        ak, bk = f"A{jb % 2}", f"A{(jb + 1) % 2}"
        sc.op("dve", lambda e, a=a, bq=bq, jb=jb: e.tensor_scalar(out=bq[:], in0=a[:], scalar1=nbf[:, jb:jb + 1], scalar2=None, op0=ALU.mult),
              reads=[ak, "nbf"], writes=[bk])
        sc.op("dve", lambda e, a=a, bq=bq, jb=jb, sh=sh: e.scalar_tensor_tensor(out=bq[:, :, sh:W], in0=a[:, :, 0:W - sh], scalar=btf[:, jb:jb + 1], in1=bq[:, :, sh:W],
                                                                              op0=ALU.mult, op1=ALU.add), reads=[ak, bk, "btf"], writes=[bk])
    Af = A[0]
    gsum = sb("gsum", [128, 8, NE], F32)
    isum = sb("isum", [128, 8, NE], F32)
    for arr, dst, dk in ((0, gateT, "gateT"), (1, isum, "isum")):
        for q in range(2):
            pb = q % 2
            for i in range(4):
                j = q * 4 + i
                sc.op("pe", lambda e, pb=pb, i=i, j=j, arr=arr: e.transpose(pT[pb][:, i * 128:(i + 1) * 128], Af[:, arr, j * 128:(j + 1) * 128], ident_f[:]),
                      inc=(i == 3), reads=["A0", "ident_f"], writes=[("pT", pb)])
            sc.op("dve", lambda e, pb=pb, q=q, dst=dst: e.tensor_reduce(out=dst[:, q * 4:(q + 1) * 4, :], in_=pT[pb][:, :].rearrange("p (i e b) -> p i e b", i=4, e=NE),
                                                                       axis=AX.X, op=ALU.add), reads=[("pT", pb)], writes=[dk, ("pT", pb)])
    flg = sb("flg", [128, 8, NE], F32)
    sc.op("dve", lambda e: e.tensor_scalar(out=flg[:], in0=gateT[:], scalar1=0.0, scalar2=None, op0=ALU.is_gt), reads=["gateT"], writes=["flg"])
    sc.op("dve", lambda e: e.tensor_scalar(out=flg[:], in0=flg[:], scalar1=-1.0, scalar2=1.0, op0=ALU.mult, op1=ALU.add), reads=["flg"], writes=["flg"])
    sc.op("dve", lambda e: e.tensor_scalar(out=flg[:], in0=flg[:], scalar1=dummyi[:, 0:1], scalar2=None, op0=ALU.mult), reads=["flg", "dummyi"], writes=["flg"])
    sc.op("dve", lambda e: e.tensor_tensor(out=isum[:], in0=isum[:], in1=flg[:], op=ALU.add), reads=["isum", "flg"], writes=["isum"])
    sc.op("dve", lambda e: e.tensor_copy(out=idxT[:], in_=isum[:]), reads=["isum"], writes=["idxT"])
    sc.barrier()
    ph.close()
    cur[0] = es

    if dbg in ("T", "FULLD"):
        dI = nc.dram_tensor("dbgI", [128, 8 * NE], U32, kind="ExternalOutput").ap()
        dGt = nc.dram_tensor("dbgGt", [128, 8 * NE], F32, kind="ExternalOutput").ap()
        dX2 = nc.dram_tensor("dbgX2", [S, D], F32, kind="ExternalOutput").ap()
        sc.dma("sp", "dbg", lambda e: e.dma_start(out=dI[:, :], in_=idxT[:].rearrange("p a b -> p (a b)")))
        sc.dma("sp", "dbg", lambda e: e.dma_start(out=dGt[:, :], in_=gateT[:].rearrange("p a b -> p (a b)")))
        sc.dma("sp", "dbg", lambda e: e.dma_start(out=dX2[:, :], in_=x2d[0:S, :]))
        if dbg == "T":
            sc.wait_all("sp")
            es.close()
            return nc
        sc.barrier()

    ph = ExitStack()
    cur[0] = ph
    NOSC = 7
    xg = sb("xg", [128, 8, D], BF16)
    xgT = sb("xgT", [128, 8, CAPH], BF16)
    actb = sb("actb", [128, 16, CAPH], BF16)
    sg = [sb(f"sg{i}", [128, 512], BF16) for i in range(2)]
    osc = [sb(f"osc{i}", [128, D], F32) for i in range(NOSC)]
    ptr = [ps(f"ptr{i}", [128, 8, 128], BF16) for i in range(2)]
    pgu = [ps(f"pgu{i}", [128, 512], F32) for i in range(4)]
    pd = [ps(f"pd{i}", [128, 512], F32) for i in range(2)]

    def issue_gather(e_):
        for j in range(NJ):
            sc.dma("pool", "xg", lambda e, j=j, e_=e_: e.indirect_dma_start(
                out=xg[:, j, :], out_offset=None, in_=h2d[:, :],
                in_offset=bass.IndirectOffsetOnAxis(ap=idxT[:, j, e_:e_ + 1], axis=0)),
                reads=["idxT"], writes=[("xg", j)])

    slots = [0, 1, 2, 3, 4, 5]
    prev_scatter = []
    gcount = 0
    ocount = 0
    tcount = 0
    issue_gather(0)
    for e_ in range(NE):
        for j in range(NJ):
            tb = tcount % 2
            tcount += 1
            for k in range(8):
                sc.op("pe", lambda e, j=j, k=k, tb=tb: e.transpose(ptr[tb][:, k, :], xg[:, j, k * 128:(k + 1) * 128], ident_b[:]),
                      inc=(k == 7), reads=[("xg", j), "ident_b"], writes=[("ptr", tb)])
            if j % 2 == 0:
                sc.op("act", lambda e, j=j, tb=tb: e.activation(out=xgT[:, :, j * 128:(j + 1) * 128], in_=ptr[tb][:, :, :], func=AF.Copy),
                      reads=[("ptr", tb)], writes=[("xgT", j), ("ptr", tb)])
            else:
                sc.op("dve", lambda e, j=j, tb=tb: e.tensor_copy(out=xgT[:, :, j * 128:(j + 1) * 128], in_=ptr[tb][:, :, :]),
                      reads=[("ptr", tb)], writes=[("xgT", j), ("ptr", tb)])
        if e_ + 1 < NE:
            issue_gather(e_ + 1)
        for h in range(2):
            sg_slot, su_slot = slots[2 * h], slots[2 * h + 1]
            for fc in range(8):
                for th, (c0, nn) in enumerate(((0, CAPH // 2), (CAPH // 2, CAPH // 2))):
                    pa = (gcount % 2) * 2
                    gcount += 1
                    rdx = [("xgT", q) for q in range(c0 // 128, (c0 + nn + 127) // 128)]
                    for k in range(8):
                        sc.op("pe", lambda e, pa=pa, k=k, fc=fc, c0=c0, nn=nn, sg_slot=sg_slot: e.matmul(
                            pgu[pa][:, 0:nn], lhsT=ring[sg_slot][:, k, fc * 128:(fc + 1) * 128], rhs=xgT[:, k, c0:c0 + nn], start=(k == 0), stop=(k == 7)),
                            inc=(k == 7), reads=rdx + [("ring", sg_slot, k)], writes=[("pgu", pa)])
                    for k in range(8):
                        sc.op("pe", lambda e, pa=pa, k=k, fc=fc, c0=c0, nn=nn, su_slot=su_slot: e.matmul(
                            pgu[pa + 1][:, 0:nn], lhsT=ring[su_slot][:, k, fc * 128:(fc + 1) * 128], rhs=xgT[:, k, c0:c0 + nn], start=(k == 0), stop=(k == 7)),
                            inc=(k == 7), reads=rdx + [("ring", su_slot, k)], writes=[("pgu", pa + 1)])
                    sb_ = (pa // 2)
                    sc.op("act", lambda e, pa=pa, sb_=sb_, nn=nn: e.activation(out=sg[sb_][:, 0:nn], in_=pgu[pa][:, 0:nn], func=AF.Silu), reads=[("pgu", pa)], writes=[("sg", sb_), ("pgu", pa)])
                    sc.op("dve", lambda e, pa=pa, sb_=sb_, h=h, fc=fc, c0=c0, nn=nn: e.tensor_tensor(
                        out=actb[:, h * 8 + fc, c0:c0 + nn], in0=pgu[pa + 1][:, 0:nn], in1=sg[sb_][:, 0:nn], op=ALU.mult),
                        reads=[("pgu", pa + 1), ("sg", sb_)], writes=[("actb", h * 8 + fc, th), ("pgu", pa + 1)])
            if e_ + 1 < NE:
                issue_weights_gu(e_ + 1, h)
        this_scatter = []
        for j in range(NJ):
            ths = [t for t in range(2) if j * 128 < (t + 1) * (CAPH // 2) and (j + 1) * 128 > t * (CAPH // 2)]
            ob = ocount % NOSC
            ocount += 1
            for n in range(2):
                for k in range(16):
                    dslot = slots[4 + k // 8]
                    sc.op("pe", lambda e, j=j, n=n, k=k, dslot=dslot: e.matmul(
                        pd[n][:, :], lhsT=actb[:, k, j * 128:(j + 1) * 128], rhs=ring[dslot][:, k % 8, n * 512:(n + 1) * 512],
                        start=(k == 0), stop=(k == 15)),
                        inc=(k == 15), reads=[("actb", k, t) for t in ths] + [("ring", dslot, k % 8)], writes=[("pd", n)])
                if n == 0:
                    sc.op("act", lambda e, ob=ob, j=j, e_=e_: e.activation(out=osc[ob][:, 0:512], in_=pd[0][:, :], func=AF.Copy, scale=gateT[:, j, e_:e_ + 1]),
                          reads=[("pd", 0), "gateT"], writes=[("osc", ob), ("pd", 0)])
                else:
                    sc.op("dve", lambda e, ob=ob, j=j, e_=e_: e.tensor_scalar(out=osc[ob][:, 512:1024], in0=pd[1][:, :], scalar1=gateT[:, j, e_:e_ + 1], scalar2=None, op0=ALU.mult),
                          reads=[("pd", 1), "gateT", ("osc", ob)], writes=[("osc", ob), ("pd", 1)])
            if j == 0:
                for t in prev_scatter:
                    sc._wait("pool", t)
            tok = sc.dma("pool", f"sca{ob}", lambda e, ob=ob, j=j, e_=e_: e.indirect_dma_start(
                out=x2d[:, :], out_offset=bass.IndirectOffsetOnAxis(ap=idxT[:, j, e_:e_ + 1], axis=0),
                in_=osc[ob][:], in_offset=None, compute_op=ALU.add, oob_is_err=True),
                reads=[("osc", ob), "idxT"], writes=[])
            this_scatter.append(tok)
        prev_scatter = this_scatter
        if e_ + 1 < NE:
            issue_weights_d(e_ + 1)
    sc.barrier()
    ph.close()
    cur[0] = es

    gfs = sb("gfs", [128, D], F32)
    sc.dma("sp", "c0", lambda e: e.dma_start(out=gfs[:], in_=gfrep[:, :]), writes=["gfs"])
    NF = 4
    fx = [sb(f"fx{i}", [128, D], F32) for i in range(NF)]
    fo = [sb(f"fo{i}", [128, D], F32) for i in range(NF)]
    fj = sb("fj", [128, D], BF16)
    fss = [sb(f"fss{i}", [128, 1], F32) for i in range(NF)]
    fsd = [sb(f"fsd{i}", [128, 1], F32) for i in range(NF)]
    frs = [sb(f"frs{i}", [128, 1], F32) for i in range(NF)]
    for ti in range(NT):
        b = ti % NF
        sc.dma("sp", f"xt{b}", lambda e, b=b, ti=ti: e.dma_start(out=fx[b][:], in_=x2d[ti * 128:(ti + 1) * 128, :]), writes=[("fx", b)])
        sc.op("act", lambda e, b=b: e.activation(out=fj[:], in_=fx[b][:], func=AF.Square, accum_out=fss[b][:]), reads=[("fx", b)], writes=["fj", ("fss", b)])
        sc.op("act", lambda e, b=b: e.activation(out=fsd[b][:], in_=fss[b][:], func=AF.Sqrt, bias=EPS, scale=1.0 / D), reads=[("fss", b)], writes=[("fsd", b)])
        sc.op("dve", lambda e, b=b: e.reciprocal(out=frs[b][:], in_=fsd[b][:]), reads=[("fsd", b)], writes=[("frs", b)])
        sc.op("dve", lambda e, b=b: e.scalar_tensor_tensor(out=fo[b][:], in0=fx[b][:], scalar=frs[b][:, 0:1], in1=gfs[:], op0=ALU.mult, op1=ALU.mult),
              reads=[("fx", b), ("frs", b), "gfs"], writes=[("fo", b)])
        sc.dma("pool", f"st{b}", lambda e, b=b, ti=ti: e.dma_start(out=out[ti * 128:(ti + 1) * 128, :], in_=fo[b][:]), reads=[("fo", b)], writes=[("out", ti)])
    sc.wait_all("sp")
    sc.wait_all("act")
    es.close()
    return nc


def host_inputs(inputs, b, half=0):
    f = np.float32
    g = lambda k: np.asarray(inputs[k], dtype=f)
    w_in = np.ascontiguousarray(g("w_in"))
    cm = np.arange(128)[:, None] * np.arange(128)[None, :]
    ang = 2.0 * np.pi * (cm % 128) / 128.0
    dft128 = np.concatenate([np.cos(ang), np.sin(ang)], axis=1).astype(f)
    wfT = np.ascontiguousarray(w_in[:, 1024:].reshape(D, 4, 128).transpose(2, 1, 0))
    rep = lambda v: np.ascontiguousarray(np.broadcast_to(g(v)[None, :], (128, D)))
    gw = np.zeros((4, 4, 128, 128), f)
    for mi, name in enumerate(["lru_wa_f", "lru_wx_f", "lru_wa_b", "lru_wx_b"]):
        w = g(name)
        for cc in range(4):
            gw[mi, cc, 0:64, 0:64] = w[2 * cc]
            gw[mi, cc, 64:128, 64:128] = w[2 * cc + 1]
    vecs = np.zeros((512, 12), f)
    vecs[:, 0:4] = g("conv_w").T
    vecs[:, 4] = g("conv_b")
    vecs[:, 5] = g("lru_ba_f"); vecs[:, 6] = g("lru_bx_f"); vecs[:, 7] = g("lru_lam_f")
    vecs[:, 8] = g("lru_ba_b"); vecs[:, 9] = g("lru_bx_b"); vecs[:, 10] = g("lru_lam_b")
    sbv = np.arange(64, dtype=np.int64)[:, None, None]
    ka = np.arange(128, dtype=np.int64)[None, :, None]
    kb = np.arange(64, dtype=np.int64)[None, None, :]
    ph = ((ka * sbv + 128 * kb * sbv) % 8192).astype(np.float64) * (2.0 * np.pi / 8192.0)
    scale = 1.0 / math.sqrt(8192.0 * 128.0)
    import ml_dtypes
    w2c = np.tile((np.cos(ph) * scale).astype(f).reshape(64, 128 * 64).astype(ml_dtypes.bfloat16), (2, 1))
    w2s = np.tile((-np.sin(ph) * scale).astype(f).reshape(64, 128 * 64).astype(ml_dtypes.bfloat16), (2, 1))
    kk = np.arange(128)
    bones = (kk[:, None] // 8 == kk[None, :] // 8).astype(f)
    mexcl = ((kk[:, None] // 8 == kk[None, :] // 8) & (kk[:, None] % 8 < kk[None, :] % 8)).astype(f)
    blkoff = ((kk % 8) * 1024).astype(f)[:, None]
    m = {
        "x": np.ascontiguousarray(inputs["x"][b], dtype=f),
        "g1rep": rep("norm1_g"), "g2rep": rep("norm2_g"), "gfrep": rep("normf_g"),
        "w_in": w_in, "wfT": wfT, "dft128": dft128, "ident": np.eye(128, dtype=f),
        "gw": np.ascontiguousarray(gw.reshape(16, 128, 128)), "vecs": vecs,
        "w2c": w2c, "w2s": w2s,
        "w_out": np.ascontiguousarray(g("w_out")), "w_router": np.ascontiguousarray(g("w_router")),
        "w_gate": np.ascontiguousarray(g("w_gate")), "w_up": np.ascontiguousarray(g("w_up")),
        "w_down": np.ascontiguousarray(g("w_down")),
        "bones": bones, "mexcl": np.ascontiguousarray(mexcl), "blkoff": blkoff,
        "halfmask": ((kk % 8) // 4 == half).astype(f)[:, None],
        "dummyidx": (S + kk).astype(f)[:, None],
    }
    return m


_CACHE = {}


def kernel(**inputs):
    nc = build_program()
    base = host_inputs(inputs, 0)
    kk = np.arange(128)
    in_maps = []
    for c in range(8):
        m = dict(base)
        m["x"] = np.ascontiguousarray(inputs["x"][c % 4], dtype=np.float32)
        m["halfmask"] = ((kk % 8) // 4 == (c // 4)).astype(np.float32)[:, None]
        in_maps.append(m)
    res = run_bass_kernel_spmd(nc, in_maps, core_ids=list(range(8)))
    H = S // 2
    outs = [np.concatenate([np.asarray(res.results[c]["out"])[:H], np.asarray(res.results[c + 4]["out"])[H:]], axis=0) for c in range(4)]
    return np.stack(outs, axis=0).astype(np.float32)
```
